# Optimizing a Trainium2 kernel written in Bass

```python
import math
import jax
import jax.numpy as jnp
from jax import lax
import numpy as np

D_MODEL = 2048
BATCH = 4
SEQ = 4096
DEPTH = 2

CHUNK = 64
N_BRANCH = 4
BRANCH = 512
CONV_W = 3
GLA_HEADS = 4
GLA_DK = 64
GLA_DV = 128
GLA_KEY = GLA_HEADS * GLA_DK
GLA_LOW_RANK = 16
GLA_LOGIT_NORM = 16.0
DIFF_HEADS = 4
DIFF_D = 64
Q_BLOCK = 128
RWKV_HEADS = 8
RWKV_HD = 64
RWKV_DECAY_LORA = 96
RWKV_AAA_LORA = 96
RWKV_MV_LORA = 64
RWKV_GATE_LORA = 256
D_FF = -(-8 * D_MODEL // (3 * 256)) * 256
RMS_EPS = 1e-6
HEAD_EPS = 1e-5
RWKV_GN_EPS = 64e-5
NEG_INF = -1e30

CONV_SIZES = (BRANCH, BRANCH, BRANCH)
GLA_SIZES = (GLA_KEY, GLA_KEY, GLA_HEADS * GLA_DV, BRANCH, GLA_LOW_RANK)
DIFF_SIZES = (DIFF_HEADS * 2 * DIFF_D,) * 3
RWKV_SIZES = (BRANCH, BRANCH, BRANCH, RWKV_DECAY_LORA, RWKV_AAA_LORA, RWKV_GATE_LORA)
GATE_SIZE = N_BRANCH * D_MODEL
TOP_SIZES = (sum(CONV_SIZES), sum(GLA_SIZES), sum(DIFF_SIZES), sum(RWKV_SIZES), GATE_SIZE)
N_IN = sum(TOP_SIZES)

kernel_name = 'hybrid_chunk_causal_gated_merge'


def _offsets(sizes):
    return [int(s) for s in np.cumsum(sizes)[:-1]]


def rms_norm(x, gain, eps=RMS_EPS):
    xf = x.astype(jnp.float32)
    y = xf * lax.rsqrt(jnp.mean(xf * xf, axis=-1, keepdims=True) + eps)
    return (y * gain.astype(jnp.float32)).astype(x.dtype)


def head_rms(o, gain, eps=HEAD_EPS):
    return o * lax.rsqrt(jnp.mean(o * o, axis=-1, keepdims=True) + eps) * gain.astype(jnp.float32)


def alibi_slopes(n_heads):
    return 2.0 ** (-8.0 * jnp.arange(1, n_heads + 1, dtype=jnp.float32) / n_heads)


def short_conv_mixer(zc, conv_w):
    gate_b, gate_c, u = jnp.split(zc, _offsets(CONV_SIZES), axis=-1)
    y = lax.conv_general_dilated(gate_c * u, conv_w[:, None, :], window_strides=(1,),
                                 padding=[(CONV_W - 1, 0)],
                                 dimension_numbers=('NWC', 'WIO', 'NWC'),
                                 feature_group_count=BRANCH)
    return gate_b * y


def gla_mixer(zg, w_a2, b_a, head_gain):
    dt = zg.dtype
    bsz, seq = zg.shape[0], zg.shape[1]
    n_chunks = seq // CHUNK
    q, k, v, g, w_lr = jnp.split(zg.astype(jnp.float32), _offsets(GLA_SIZES), axis=-1)
    log_a = jax.nn.log_sigmoid(w_lr @ w_a2 + b_a) / GLA_LOGIT_NORM

    def chunks(t, d):
        return t.reshape(bsz, n_chunks, CHUNK, GLA_HEADS, d).transpose(1, 0, 3, 2, 4)

    qc = chunks(q * GLA_DK ** -0.5, GLA_DK)
    kc = chunks(k, GLA_DK)
    vc = chunks(v, GLA_DV)
    ac = chunks(log_a, GLA_DK)
    causal = jnp.tril(jnp.ones((CHUNK, CHUNK), dtype=bool))[:, :, None]

    def step(state, inp):
        qi, ki, vi, ai = inp
        cum = jnp.cumsum(ai, axis=2)
        rel = cum[:, :, :, None, :] - cum[:, :, None, :, :]
        decay = jnp.where(causal, jnp.exp(jnp.minimum(rel, 0.0)), 0.0)
        scores = jnp.einsum('bhtd,bhtsd,bhsd->bhts', qi, decay, ki)
        out = scores @ vi + (qi * jnp.exp(cum)) @ state
        last = cum[:, :, -1:, :]
        state = (jnp.exp(last[:, :, 0, :, None]) * state
                 + jnp.einsum('bhsd,bhse->bhde', ki * jnp.exp(last - cum), vi))
        return state, out

    state0 = jnp.zeros((bsz, GLA_HEADS, GLA_DK, GLA_DV), jnp.float32)
    _, o = lax.scan(step, state0, (qc, kc, vc, ac))
    o = o.transpose(1, 0, 3, 2, 4).reshape(bsz, seq, GLA_HEADS, GLA_DV)
    o = head_rms(o, head_gain).reshape(bsz, seq, BRANCH) * jax.nn.silu(g)
    return o.astype(dt)


def diff_attention_mixer(zd, lq1, lk1, lq2, lk2, head_gain, lambda_init):
    dt = zd.dtype
    bsz, seq = zd.shape[0], zd.shape[1]
    n_blocks = seq // Q_BLOCK
    f32 = jnp.float32
    q, k, v = jnp.split(zd.astype(f32), _offsets(DIFF_SIZES), axis=-1)
    q = q.reshape(bsz, seq, DIFF_HEADS, 2, DIFF_D) * DIFF_D ** -0.5
    k = k.reshape(bsz, seq, DIFF_HEADS, 2, DIFF_D)
    v = v.reshape(bsz, seq, DIFF_HEADS, 2 * DIFF_D)
    lam = (jnp.exp(jnp.sum(lq1.astype(f32) * lk1.astype(f32)))
           - jnp.exp(jnp.sum(lq2.astype(f32) * lk2.astype(f32))) + lambda_init)
    slopes = alibi_slopes(DIFF_HEADS)[:, None, None]
    k_pos = jnp.arange(seq)
    q_blocks = q.reshape(bsz, n_blocks, Q_BLOCK, DIFF_HEADS, 2, DIFF_D).transpose(1, 0, 2, 3, 4, 5)

    def block(args):
        qi, bi = args
        q_pos = bi * Q_BLOCK + jnp.arange(Q_BLOCK)
        s = jnp.einsum('bqhmd,bkhmd->bhmqk', qi, k)
        dist = jnp.abs(q_pos[:, None] - k_pos[None, :]).astype(f32)
        visible = (k_pos[None, :] // CHUNK) <= (q_pos[:, None] // CHUNK)
        bias = jnp.where(visible[None], -slopes * dist[None], NEG_INF)
        p = jax.nn.softmax(s + bias[None, :, None], axis=-1)
        attn = p[:, :, 0] - lam * p[:, :, 1]
        return jnp.einsum('bhqk,bkhe->bqhe', attn, v)

    o = lax.map(block, (q_blocks, jnp.arange(n_blocks)))
    o = o.transpose(1, 0, 2, 3, 4).reshape(bsz, seq, DIFF_HEADS, 2 * DIFF_D)
    o = head_rms(o, head_gain) * (1.0 - lambda_init)
    return o.reshape(bsz, seq, BRANCH).astype(dt)


def rwkv7_mixer(zr, mu, w0, w2, a0, a2, g2, k_k, k_a, r_k, ln_w, ln_b, v_first, vres):
    dt = zr.dtype
    bsz, seq = zr.shape[0], zr.shape[1]
    zr = zr.astype(jnp.float32)
    prev = jnp.pad(zr, ((0, 0), (1, 0), (0, 0)))[:, :-1]
    zm = zr + (prev - zr) * mu
    r, k, v, w_lr, a_lr, g_lr = jnp.split(zm, _offsets(RWKV_SIZES), axis=-1)
    log_w = -jax.nn.softplus(-(w0 + jnp.tanh(w_lr) @ w2)) - 0.5
    decay = jnp.exp(-jnp.exp(log_w))
    a = jax.nn.sigmoid(a0 + a_lr @ a2)
    g = jax.nn.sigmoid(g_lr) @ g2
    if vres is None:
        v_first = v
    else:
        v0, v1, v2 = vres
        v = v + (v_first - v) * jax.nn.sigmoid(v0 + (v @ v1) @ v2)

    def heads(t):
        return t.reshape(bsz, seq, RWKV_HEADS, RWKV_HD)

    kk = heads(k * k_k)
    kk = kk / jnp.maximum(jnp.sqrt(jnp.sum(kk * kk, axis=-1, keepdims=True)), 1e-12)
    k = k * (1.0 + (a - 1.0) * k_a)
    rh, kh, vh, wh, ah = heads(r), heads(k), heads(v), heads(decay), heads(a)

    def step(state, inp):
        r_t, w_t, k_t, v_t, kk_t, b_t = inp
        sa = jnp.einsum('bhvk,bhk->bhv', state, -kk_t)
        state = (state * w_t[:, :, None, :] + sa[..., None] * b_t[:, :, None, :]
                 + v_t[..., None] * k_t[:, :, None, :])
        return state, jnp.einsum('bhvk,bhk->bhv', state, r_t)

    xs = tuple(jnp.swapaxes(t, 0, 1) for t in (rh, wh, kh, vh, kk, kk * ah))
    state0 = jnp.zeros((bsz, RWKV_HEADS, RWKV_HD, RWKV_HD), jnp.float32)
    _, o = lax.scan(step, state0, xs)
    o = jnp.swapaxes(o, 0, 1)
    mean = jnp.mean(o, axis=-1, keepdims=True)
    var = jnp.mean(jnp.square(o - mean), axis=-1, keepdims=True)
    o = ((o - mean) * lax.rsqrt(var + RWKV_GN_EPS)).reshape(bsz, seq, BRANCH) * ln_w + ln_b
    bonus = jnp.sum(rh * kh * r_k.reshape(RWKV_HEADS, RWKV_HD), axis=-1, keepdims=True) * vh
    out = (o + bonus.reshape(bsz, seq, BRANCH)) * g
    return out.astype(dt), v_first


def setup_inputs(seed: int = 0) -> dict:
    key = jax.random.key(seed)
    ks = iter(jax.random.split(key, 48))
    f32 = jnp.float32
    L = DEPTH

    def nrm(shape, scale):
        return jax.random.normal(next(ks), shape, f32) * scale

    def gain(shape):
        return 1.0 + nrm(shape, 0.02)

    return {
        'x': nrm((BATCH, SEQ, D_MODEL), 1.0),
        'norm_mix_pre': gain((L, D_MODEL)),
        'w_in': nrm((L, D_MODEL, N_IN), D_MODEL ** -0.5),
        'conv_w': nrm((L, CONV_W, BRANCH), CONV_W ** -0.5),
        'gla_wa2': nrm((L, GLA_LOW_RANK, GLA_KEY), GLA_LOW_RANK ** -0.5),
        'gla_ba': nrm((L, GLA_KEY), 0.01),
        'gla_norm': gain((L, GLA_DV)),
        'diff_lq1': nrm((L, DIFF_D), 0.1),
        'diff_lk1': nrm((L, DIFF_D), 0.1),
        'diff_lq2': nrm((L, DIFF_D), 0.1),
        'diff_lk2': nrm((L, DIFF_D), 0.1),
        'diff_norm': gain((L, 2 * DIFF_D)),
        'rw_mu': jax.random.uniform(next(ks), (L, sum(RWKV_SIZES)), f32),
        'rw_w0': jax.random.uniform(next(ks), (L, BRANCH), f32, -6.0, -1.0),
        'rw_w2': nrm((L, RWKV_DECAY_LORA, BRANCH), 0.1 * RWKV_DECAY_LORA ** -0.5),
        'rw_a0': nrm((L, BRANCH), 0.1),
        'rw_a2': nrm((L, RWKV_AAA_LORA, BRANCH), 0.5 * RWKV_AAA_LORA ** -0.5),
        'rw_g2': nrm((L, RWKV_GATE_LORA, BRANCH), RWKV_GATE_LORA ** -0.5),
        'rw_kk': 0.85 + nrm((L, BRANCH), 0.02),
        'rw_ka': gain((L, BRANCH)),
        'rw_rk': nrm((L, BRANCH), 0.1),
        'rw_lnw': gain((L, BRANCH)),
        'rw_lnb': nrm((L, BRANCH), 0.01),
        'rw_v0': 1.0 + nrm((L - 1, BRANCH), 0.1),
        'rw_v1': nrm((L - 1, BRANCH, RWKV_MV_LORA), BRANCH ** -0.5),
        'rw_v2': nrm((L - 1, RWKV_MV_LORA, BRANCH), 0.5 * RWKV_MV_LORA ** -0.5),
        'w_branch': nrm((L, N_BRANCH, BRANCH, D_MODEL), BRANCH ** -0.5),
        'w_out': nrm((L, D_MODEL, D_MODEL), D_MODEL ** -0.5),
        'norm_mix_post': gain((L, D_MODEL)),
        'norm_ffn_pre': gain((L, D_MODEL)),
        'w_gate': nrm((L, D_MODEL, D_FF), D_MODEL ** -0.5),
        'w_up': nrm((L, D_MODEL, D_FF), D_MODEL ** -0.5),
        'w_down': nrm((L, D_FF, D_MODEL), D_FF ** -0.5),
        'norm_ffn_post': gain((L, D_MODEL)),
    }


def reference(x, norm_mix_pre, w_in, conv_w, gla_wa2, gla_ba, gla_norm, diff_lq1, diff_lk1,
              diff_lq2, diff_lk2, diff_norm, rw_mu, rw_w0, rw_w2, rw_a0, rw_a2, rw_g2, rw_kk,
              rw_ka, rw_rk, rw_lnw, rw_lnb, rw_v0, rw_v1, rw_v2, w_branch, w_out, norm_mix_post,
              norm_ffn_pre, w_gate, w_up, w_down, norm_ffn_post):
    bsz, seq = x.shape[0], x.shape[1]
    v_first = None
    for l in range(DEPTH):
        h = rms_norm(x, norm_mix_pre[l])
        z = h @ w_in[l]
        z_conv, z_gla, z_diff, z_rwkv, z_gate = jnp.split(z, _offsets(TOP_SIZES), axis=-1)
        lambda_init = 0.8 - 0.6 * math.exp(-0.3 * l)
        vres = None if l == 0 else (rw_v0[l - 1], rw_v1[l - 1], rw_v2[l - 1])
        o_conv = short_conv_mixer(z_conv, conv_w[l])
        o_gla = gla_mixer(z_gla, gla_wa2[l], gla_ba[l], gla_norm[l])
        o_diff = diff_attention_mixer(z_diff, diff_lq1[l], diff_lk1[l], diff_lq2[l], diff_lk2[l],
                                      diff_norm[l], lambda_init)
        o_rwkv, v_first = rwkv7_mixer(z_rwkv, rw_mu[l], rw_w0[l], rw_w2[l], rw_a0[l], rw_a2[l],
                                      rw_g2[l], rw_kk[l], rw_ka[l], rw_rk[l], rw_lnw[l], rw_lnb[l],
                                      v_first, vres)
        gates = jax.nn.sigmoid(z_gate.reshape(bsz, seq, N_BRANCH, D_MODEL))
        merged = gates[:, :, 0] * (o_conv @ w_branch[l, 0])
        for n, o_n in ((1, o_gla), (2, o_diff), (3, o_rwkv)):
            merged = merged + gates[:, :, n] * (o_n @ w_branch[l, n])
        x = x + rms_norm(merged @ w_out[l], norm_mix_post[l])
        h = rms_norm(x, norm_ffn_pre[l])
        ffn = (jax.nn.silu(h @ w_gate[l]) * (h @ w_up[l])) @ w_down[l]
        x = x + rms_norm(ffn, norm_ffn_post[l])
    return x
```

```python
import math
import numpy as np
from contextlib import ExitStack
import concourse.bass as bass
import concourse.mybir as mybir
from concourse.bass_utils import run_bass_kernel_spmd

F32 = mybir.dt.float32
BF16 = mybir.dt.bfloat16
AF = mybir.ActivationFunctionType
ALU = mybir.AluOpType

D = 2048
SEQ = 4096
NIN = 14800
DFF = 5632
TT = 512
NKC = 16
C_CB, C_CC, C_CU = 0, 512, 1024
G_Q, G_K, G_V, G_G, G_W = 1536, 1792, 2048, 2560, 3072
DQ, DK, DV = 3088, 3600, 4112
R_R, R_K, R_V, R_W, R_A, R_G = 4624, 5136, 5648, 6160, 6256, 6352
C_GATE = 6608
S0 = math.exp(-0.5)
SLOPES = [2.0 ** (-8.0 * (i + 1) / 4) for i in range(4)]

P_GPRE, P_GFFN, P_CONV, P_GLAN, P_DIFN = 0, 16, 32, 44, 45
P_MUR, P_MUK, P_MUV, P_MUW, P_MUA, P_MUG = 46, 50, 54, 58, 59, 60
P_A0, P_KK, P_KA, P_RK, P_LNW, P_LNB, P_V0 = 62, 66, 70, 74, 78, 82, 86
P_LQ = 90
NPAR = 346
RW_BA, RW_W0, NROW = 0, 256, 768
K_ID, K_LE, K_LT, K_GT, K_MEAN, K_BD, K_ONE, K_BDIAG, K_BTAB = 0, 128, 256, 384, 512, 640, 768, 896, 1408
NCST = 1408 + 128


class Ctx:
    def __init__(self, nc, es):
        self.nc = nc
        self.es = es
        self.eng = {}
        for nm, h in (("pe", nc.tensor), ("act", nc.scalar), ("dve", nc.vector), ("pool", nc.gpsimd), ("sp", nc.sync)):
            self.eng[nm] = dict(h=h, sem=es.enter_context(nc.semaphore("s_" + nm)), n=0, seen={}, name=nm, mul=1)
        self.chans = {}
        self.bufs = {}
        self.nsem = 5
        self.ninst = 0

    def buf(self, ap):
        nm = ap.tensor.name
        b = self.bufs.get(nm)
        if b is None:
            b = dict(w=None, r={})
            self.bufs[nm] = b
        return b

    def chan(self, key):
        c = self.chans.get(key)
        if c is None:
            c = dict(sem=self.es.enter_context(self.nc.semaphore("c_" + key)), n=0, name="c_" + key, mul=16)
            self.chans[key] = c
            self.nsem += 1
        return c

    def _wait(self, e, src, idx):
        if src is e and e["name"] == "pe":
            return
        if src is e and e["name"] == "sp":
            return
        k = src["name"]
        if e["seen"].get(k, 0) >= idx:
            return
        e["h"].wait_ge(src["sem"], idx * src["mul"])
        e["seen"][k] = idx

    def _deps(self, e, R, W):
        for ap in R:
            b = self.buf(ap)
            if b["w"] is not None:
                self._wait(e, *b["w"])
        for ap in W:
            b = self.buf(ap)
            if b["w"] is not None:
                self._wait(e, *b["w"])
            for (src, idx) in b["r"].values():
                self._wait(e, src, idx)

    def _mark(self, src, R, W):
        idx = src["n"]
        for ap in R:
            self.buf(ap)["r"][src["name"]] = (src, idx)
        for ap in W:
            b = self.buf(ap)
            b["w"] = (src, idx)
            b["r"] = {}

    def op(self, en, fn, R, W):
        e = self.eng[en]
        R = [a for a in R if a is not None and not isinstance(a, (int, float))]
        self._deps(e, R, W)
        inst = fn(e["h"])
        e["n"] += 1
        inst.then_inc(e["sem"], 1)
        self._mark(e, R, W)
        self.ninst += 1

    def dma(self, qn, out, in_, key=None):
        e = self.eng[qn]
        if key is None:
            key = out.tensor.name if "DRam" not in type(out.tensor).__name__ else in_.tensor.name
            if key[0] == "l" and "_" in key and key[1:key.index("_")].isdigit():
                key = key[key.index("_") + 1:]
        c = self.chan(key)
        self._deps(e, [in_], [out])
        inst = e["h"].dma_start(out=out, in_=in_)
        c["n"] += 1
        inst.then_inc(c["sem"], 16)
        self._mark(c, [in_], [out])
        self.ninst += 1

    def barrier(self):
        srcs = list(self.eng.values()) + list(self.chans.values())
        for e in self.eng.values():
            for s in srcs:
                if s is e or s["n"] == 0:
                    continue
                self._wait(e, s, s["n"])

    def mm(self, out, lhsT, rhs, start=True, stop=True):
        self.op("pe", lambda h: h.matmul(out, lhsT=lhsT, rhs=rhs, start=start, stop=stop, skip_group_check=True),
                [lhsT, rhs], [out])

    def tr(self, out, in_, ident):
        self.op("pe", lambda h: h.transpose(out, in_, ident), [in_, ident], [out])

    def act(self, out, in_, func, bias=None, scale=None, en="act"):
        kw = {}
        if bias is not None:
            kw["bias"] = bias
        if scale is not None:
            kw["scale"] = scale
        self.op(en, lambda h: h.activation(out=out, in_=in_, func=func, **kw), [in_, bias, scale], [out])

    def tt(self, out, a, b, op, en="dve"):
        self.op(en, lambda h: h.tensor_tensor(out=out, in0=a, in1=b, op=op), [a, b], [out])

    def ts(self, out, a, s1, op0, s2=None, op1=None, en="dve"):
        if op1 is None:
            self.op(en, lambda h: h.tensor_scalar(out=out, in0=a, scalar1=s1, scalar2=None, op0=op0), [a, s1], [out])
        else:
            self.op(en, lambda h: h.tensor_scalar(out=out, in0=a, scalar1=s1, scalar2=s2, op0=op0, op1=op1),
                    [a, s1, s2], [out])

    def stt(self, out, a, s, b, op0, op1):
        self.op("dve", lambda h: h.scalar_tensor_tensor(out=out, in0=a, scalar=s, in1=b, op0=op0, op1=op1),
                [a, s, b], [out])

    def copy(self, out, in_, en="dve"):
        if en == "act":
            self.act(out, in_, AF.Copy)
        else:
            self.op(en, lambda h: h.tensor_copy(out=out, in_=in_), [in_], [out])

    def recip(self, out, in_):
        self.op("dve", lambda h: h.reciprocal(out=out, in_=in_), [in_], [out])

    def memset(self, ap, v, en="pool"):
        self.op(en, lambda h: h.memset(ap, v), [], [ap])


def build(nc, n_tiles=8, layers=(0, 1), dbg=(), stage=99):
    L = len(layers)
    es = ExitStack()
    K = Ctx(nc, es)
    dumps = {}

    def dram_in(name, shape, dt=F32):
        return nc.dram_tensor(name, list(shape), dt, kind="ExternalInput").ap()

    def dram_scr(name, shape, dt):
        return nc.dram_tensor("X_" + name, list(shape), dt, kind="Internal").ap()

    x_in = dram_in("x", [SEQ, D])
    w_in = {l: [dram_in(f"win{l}_{j}", [256, NIN]) for j in range(8)] for l in layers}
    w_br = {l: [dram_in(f"wbr{l}_{n}", [512, D]) for n in range(4)] for l in layers}
    w_out = {l: [dram_in(f"wout{l}_{j}", [1024, D]) for j in range(2)] for l in layers}
    w_gate = {l: [dram_in(f"wg{l}_{j}", [512, DFF]) for j in range(4)] for l in layers} if stage >= 7 else None
    w_up = {l: [dram_in(f"wu{l}_{j}", [512, DFF]) for j in range(4)] for l in layers} if stage >= 7 else None
    w_down = {l: [dram_in(f"wd{l}_{j}", [1408, D]) for j in range(4)] for l in layers} if stage >= 7 else None
    cst_d = dram_in("cst", [128, NCST])
    par_d = dram_in("par", [2, 128, NPAR])
    row_d = dram_in("rows", [2, 1, NROW])
    gpost_d = dram_in("gpost_bc", [2, 128, D])
    gfpost_d = dram_in("gfpost_bc", [2, 128, D])
    wa2_d = dram_in("gla_wa2", [2, 16, 256])
    w2_d = dram_in("rw_w2", [2, 96, 512])
    a2_d = dram_in("rw_a2", [2, 96, 512])
    g2_d = dram_in("rw_g2", [2, 256, 512])
    v1_d = dram_in("rw_v1", [1, 512, 64])
    v2_d = dram_in("rw_v2", [1, 64, 512])
    y_out = nc.dram_tensor("y", [SEQ, D], F32, kind="ExternalOutput").ap()

    s_win = [dram_scr(f"win{l}", [128, NKC, NIN], BF16) for l in range(2)]
    s_wbr = [[dram_scr(f"wbr{l}_{n}", [128, 4, D], BF16) for n in range(4)] for l in range(2)]
    s_wout = [dram_scr(f"wout{l}", [128, NKC, D], BF16) for l in range(2)]
    s_wg = [dram_scr(f"wg{l}", [128, NKC, DFF], BF16) for l in range(2)]
    s_wu = [dram_scr(f"wu{l}", [128, NKC, DFF], BF16) for l in range(2)]
    s_wd = [dram_scr(f"wd{l}", [128, 44, D], BF16) for l in range(2)]
    s_x1 = dram_scr("x1", [SEQ, D], F32)
    s_xm = dram_scr("xm", [TT, D], F32)
    s_vf = dram_scr("vf", [128, 4, SEQ], F32)

    uid = [0]

    def sb(name, shape, dt=F32):
        return es.enter_context(nc.sbuf_tensor("sb_" + name, list(shape), dt))

    def loc(e_, name, shape, dt=F32):
        uid[0] += 1
        return e_.enter_context(nc.sbuf_tensor(f"l{uid[0]}_{name}", list(shape), dt))

    def ps(name, dt=F32):
        return es.enter_context(nc.psum_tensor("ps_" + name, [128, 512], dt))

    cst = sb("cst", [128, NCST])
    identb = sb("identb", [128, 128], BF16)
    onesb = sb("onesb", [128, 128], BF16)
    par = sb("par", [128, NPAR])
    rows = sb("rows", [1, NROW])
    wa2 = sb("wa2", [16, 256])
    w2 = sb("w2", [96, 512])
    a2 = sb("a2", [96, 512])
    g2 = sb("g2", [128, 2, 512])
    v1 = sb("v1", [128, 4, 64])
    v2 = sb("v2", [64, 512])
    hT = [sb(f"hT{kc}", [128, TT + 1], BF16) for kc in range(NKC)]
    halo = sb("halo", [128, NKC], BF16)
    KS = [dram_scr(f"KS{h}", [128, SEQ], BF16) for h in range(4)]
    VS = [dram_scr(f"VS{h}", [128, SEQ // 128, 128], BF16) for h in range(4)]
    NSLOT = 3
    wslot = [sb(f"wslot{i}", [128, NKC, 528], BF16) for i in range(NSLOT)]
    cu_halo = sb("cu_halo", [128, 4, 2])
    gst = [sb(f"gst{i}", [128, 2, 256]) for i in range(2)]
    rst = [sb(f"rst{i}", [128, 4, 64]) for i in range(2)]
    lam = sb("lam", [128, 4])
    pb = [ps(f"pb{i}") for i in range(7)]
    ptt = es.enter_context(nc.psum_tensor("ps_pt", [128, 1024], BF16))
    pt = [ptt[:, 0:512], ptt[:, 512:1024]]

    ident = cst[:, K_ID:K_ID + 128]
    mLE = cst[:, K_LE:K_LE + 128]
    mLT = cst[:, K_LT:K_LT + 128]
    mGT = cst[:, K_GT:K_GT + 128]
    omean = cst[:, K_MEAN:K_MEAN + 128]
    bd64 = cst[:, K_BD:K_BD + 128]
    ones = cst[:, K_ONE:K_ONE + 128]

    def dump(name, ap, shape, dt=F32):
        if name not in dbg:
            return
        t = nc.dram_tensor("dbg_" + name, list(shape), dt, kind="ExternalOutput").ap()
        K.dma("pool", t, ap, key="dbg_" + name)
        dumps[name] = t

    K.dma("pool", cst[:, :], cst_d[:, :])
    K.dma("pool", identb[:, :], cst_d[:, K_ID:K_ID + 128])
    K.dma("pool", onesb[:, :], cst_d[:, K_ONE:K_ONE + 128])

    cast_keys = []

    def cast(dst, src):
        k = f"cast{len(cast_keys)}"
        if len(cast_keys) >= 2:
            c = K.chans[cast_keys[-2]]
            K._wait(K.eng["pool"], c, c["n"])
        cast_keys.append(k)
        K.dma("pool", dst, src, key=k)

    cast_q = []

    def cast_layer(l):
        r = lambda ap: ap.rearrange("(k p) n -> p k n", p=128)
        q = []
        for j in range(8):
            q.append((s_win[l][:, 2 * j:2 * j + 2, :], r(w_in[l][j])))
        for n in range(4):
            q.append((s_wbr[l][n][:, :, :], r(w_br[l][n])))
        for j in range(2):
            q.append((s_wout[l][:, 8 * j:8 * j + 8, :], r(w_out[l][j])))
        if stage >= 7:
            for j in range(4):
                q.append((s_wg[l][:, 4 * j:4 * j + 4, :], r(w_gate[l][j])))
                q.append((s_wu[l][:, 4 * j:4 * j + 4, :], r(w_up[l][j])))
            for j in range(4):
                q.append((s_wd[l][:, 11 * j:11 * j + 11, :], r(w_down[l][j])))
        return q

    def emit_casts(n):
        for _ in range(min(n, len(cast_q))):
            d_, s_ = cast_q.pop(0)
            cast(d_, s_)

    ncast_layer = 0
    for l in layers:
        q = cast_layer(l)
        ncast_layer = len(q)
        cast_q += q
    emit_casts(8)

    class WS:
        def __init__(self):
            self.plan = []
            self.issued = 0
            self.cur = 0

        def issue(self, upto):
            while self.issued < min(upto, len(self.plan)):
                g = self.plan[self.issued]
                slot = wslot[self.issued % NSLOT]
                for (dst_c, src, nk, nco) in g[1]:
                    assert K.buf(src)["w"] is not None, g[0]
                    K.dma("sp", slot[:, 0:nk, dst_c:dst_c + nco], src, key=f"ws{self.issued % NSLOT}")
                self.issued += 1

        def next(self, key):
            g = self.plan[self.cur]
            assert g[0] == key, (g[0], key)
            self.issue(self.cur + NSLOT - 1)
            slot = wslot[self.cur % NSLOT]
            self.cur += 1
            return slot

    W = WS()

    def plan_layer(l, ti):
        p = []
        sw = s_win[l]

        def g(key, *pieces):
            lst = []
            dc = 0
            for (c0, nco) in pieces:
                lst.append((dc, sw[:, :, c0:c0 + nco], NKC, nco))
                dc += nco
            p.append((key, lst))
        g("cC", (C_CC, 512)); g("cU", (C_CU, 512)); g("cB", (C_CB, 512))
        if stage >= 3:
            g("gQK", (G_Q, 512)); g("gGW", (G_G, 528)); g("gV", (G_V, 512))
        if stage >= 4:
            g("dQ", (DQ, 512)); g("dK", (DK, 512)); g("dV", (DV, 512))
        if stage >= 5:
            g("rV", (R_V, 512)); g("rL", (R_W, 448))
            for hp in range(4):
                g(f"rRK{hp}", (R_R + hp * 128, 128), (R_K + hp * 128, 128))
        if stage >= 6:
            for jq in range(4):
                for n in range(4):
                    g(f"mg{jq}{n}", (C_GATE + n * D + jq * 512, 512))
                    p.append((f"mb{jq}{n}", [(0, s_wbr[l][n][:, :, jq * 512:(jq + 1) * 512], 4, 512)]))
            for cg in range(4):
                p.append((f"wo{cg}", [(0, s_wout[l][:, :, cg * 512:(cg + 1) * 512], NKC, 512)]))
        if stage >= 7:
            for j in range(11):
                p.append((f"fg{j}", [(0, s_wg[l][:, :, j * 512:(j + 1) * 512], NKC, 512)]))
                p.append((f"fu{j}", [(0, s_wu[l][:, :, j * 512:(j + 1) * 512], NKC, 512)]))
            for cg in range(4):
                for kg, (k0, nk) in enumerate(((0, 16), (16, 16), (32, 12))):
                    p.append((f"fd{cg}{kg}", [(0, s_wd[l][:, k0:k0 + nk, cg * 512:(cg + 1) * 512], nk, 512)]))
        return p

    for l in layers:
        for ti in range(n_tiles):
            W.plan += plan_layer(l, ti)

    def proj_fm(pst, slot, c0, ncols, shift=0):
        for kc in range(NKC):
            K.mm(pst[0:ncols, :], lhsT=slot[:, kc, c0:c0 + ncols], rhs=hT[kc][:, 1 - shift:TT + 1 - shift],
                 start=(kc == 0), stop=(kc == NKC - 1))

    def proj_tm(pst, slot, c0, ncols, ts_, shift=0):
        o = 1 - shift + ts_ * 128
        for kc in range(NKC):
            K.mm(pst[:, 0:ncols], lhsT=hT[kc][:, o:o + 128], rhs=slot[:, kc, c0:c0 + ncols],
                 start=(kc == 0), stop=(kc == NKC - 1))

    def rstd_from_ss(dst, ssq, n, eps):
        K.act(dst, ssq, AF.Sqrt, bias=epsb[:, eps:eps + 1], scale=1.0 / n)
        K.recip(dst, dst)

    epsb = sb("epsb", [128, 4])
    K.memset(epsb[:, 0:1], 1e-6)
    K.memset(epsb[:, 1:2], 1e-5)
    K.memset(epsb[:, 2:3], 64e-5)
    K.memset(epsb[:, 3:4], 1.0)

    def norm_transpose(src_fn, gcol, es2, tag):
        xn = loc(es2, f"xn_{tag}", [128, 4, D], BF16)
        ss = loc(es2, f"ss_{tag}", [128, 4])
        for ts_ in range(4):
            xt = src_fn(ts_)
            K.op("act", lambda h: h.activation(out=xn[:, ts_, :], in_=xt, func=AF.Square, accum_out=ss[:, ts_:ts_ + 1]),
                 [xt], [xn[:, ts_, :], ss[:, :]])
            rstd_from_ss(ss[:, ts_:ts_ + 1], ss[:, ts_:ts_ + 1], D, 0)
            K.ts(xn[:, ts_, :], xt, ss[:, ts_:ts_ + 1], ALU.mult)
        for kc in range(NKC):
            pp = pt[kc % 2]
            for ts_ in range(4):
                K.tr(pp[:, ts_ * 128:(ts_ + 1) * 128], xn[:, ts_, kc * 128:(kc + 1) * 128], identb[:, :])
            if kc % 2 == 0:
                K.ts(hT[kc][:, 1:TT + 1], pp, par[:, gcol + kc:gcol + kc + 1], ALU.mult)
            else:
                K.act(hT[kc][:, 1:TT + 1], pp, AF.Copy, scale=par[:, gcol + kc:gcol + kc + 1])


    gsi = [0, 0]
    rsi = {}
    rot = [0]

    def rps(lo=3, hi=7):
        rot[0] += 1
        return pb[lo + rot[0] % (hi - lo)]

    def ev(i, out, in_):
        if i % 2 == 0:
            K.act(out, in_, AF.Copy)
        else:
            K.copy(out, in_)

    def gla_phase(ti, oT):
        with ExitStack() as e3:
            qT = loc(e3, "qT", [128, 2, TT]); kT = loc(e3, "kT", [128, 2, TT])
            ktm = loc(e3, "ktm", [128, 4, 256]); vtm = loc(e3, "gvtm", [128, 4, 512])
            sgT = loc(e3, "sgT", [128, 4, TT], BF16); wlrT = loc(e3, "wlrT", [16, TT])
            Ltm = loc(e3, "Ltm", [128, 4, 256]); Eq = loc(e3, "Eq", [128, 2, TT]); Ek = loc(e3, "Ek", [128, 2, TT])
            kd = loc(e3, "kd", [128, 4, 256]); sc = [loc(e3, f"sc{i}", [128, 128]) for i in range(2)]
            osb = loc(e3, "osb", [128, TT]); sq = loc(e3, "sq", [128, TT]); rs = loc(e3, "rs", [128, TT])
            slot = W.next("gQK")
            for hp in range(2):
                p_ = pb[hp]; proj_fm(p_, slot, hp * 128, 128)
                K.act(qT[:, hp, :], p_[:, :], AF.Copy, scale=0.125)
            for hp in range(2):
                p_ = pb[2 + hp]; proj_fm(p_, slot, 256 + hp * 128, 128)
                K.act(kT[:, hp, :], p_[:, :], AF.Copy)
            for ts_ in range(4):
                p_ = pb[4 + ts_ % 2]; proj_tm(p_, slot, 256, 256, ts_)
                ev(ts_, ktm[:, ts_, :], p_[:, 0:256])
            slot = W.next("gGW")
            for h in range(4):
                p_ = pb[h % 2]; proj_fm(p_, slot, h * 128, 128)
                K.act(sgT[:, h, :], p_[:, :], AF.Silu)
            p_ = pb[2]; proj_fm(p_, slot, 512, 16)
            K.act(wlrT[:, :], p_[0:16, :], AF.Copy)
            slot = W.next("gV")
            for ts_ in range(4):
                p_ = pb[3 + ts_ % 2]; proj_tm(p_, slot, 0, 512, ts_)
                ev(ts_, vtm[:, ts_, :], p_[:, :])
            for ts_ in range(4):
                p_ = pb[5 + ts_ % 2]
                K.mm(p_[:, 0:256], lhsT=wlrT[0:16, ts_ * 128:(ts_ + 1) * 128], rhs=wa2[0:16, :], start=True, stop=False)
                K.mm(p_[:, 0:256], lhsT=ones[0:1, :], rhs=rows[0:1, RW_BA:RW_BA + 256], start=False, stop=True)
                K.act(Ltm[:, ts_, :], p_[:, 0:256], AF.Exp, scale=-1.0)
                K.act(Ltm[:, ts_, :], Ltm[:, ts_, :], AF.Ln, bias=epsb[:, 3:4])
            for hp in range(2):
                p_ = pb[hp]
                for ts_ in range(4):
                    K.mm(p_[:, ts_ * 128:(ts_ + 1) * 128], lhsT=Ltm[:, ts_, hp * 128:(hp + 1) * 128], rhs=mLE)
                K.act(Eq[:, hp, :], p_[:, :], AF.Exp, scale=-1.0 / 16)
                K.act(Ek[:, hp, :], p_[:, :], AF.Exp, scale=1.0 / 16)
                K.tt(qT[:, hp, :], qT[:, hp, :], Eq[:, hp, :], ALU.mult)
                K.tt(kT[:, hp, :], kT[:, hp, :], Ek[:, hp, :], ALU.mult)
            for ts_ in range(4):
                p_ = pb[2 + ts_ % 2]
                K.mm(p_[:, 0:256], lhsT=mGT, rhs=Ltm[:, ts_, :])
                K.act(kd[:, ts_, :], p_[:, 0:256], AF.Exp, scale=-1.0 / 16)
                K.tt(kd[:, ts_, :], kd[:, ts_, :], ktm[:, ts_, :], ALU.mult)
            for hp in range(2):
                po = [pb[0], pb[1]]
                for ts_ in range(4):
                    c0 = ts_ * 128
                    for j in range(2):
                        hs = slice(64 * j, 64 * j + 64); h = 2 * hp + j
                        ps_ = pb[2 + j]
                        K.mm(ps_[:, 0:128], lhsT=kT[hs, hp, c0:c0 + 128], rhs=qT[hs, hp, c0:c0 + 128])
                        K.tt(sc[j][:, :], ps_[:, 0:128], mLE, ALU.mult)
                        K.mm(po[j][:, c0:c0 + 128], lhsT=vtm[:, ts_, h * 128:(h + 1) * 128], rhs=sc[j][:, :], start=True, stop=False)
                    for cc in range(2):
                        cs = c0 + cc * 64
                        st = gst[gsi[hp]]; st2 = gst[1 - gsi[hp]]
                        for j in range(2):
                            hs = slice(64 * j, 64 * j + 64)
                            K.mm(po[j][:, cs:cs + 64], lhsT=st[hs, hp, 128 * j:128 * j + 128], rhs=qT[hs, hp, cs:cs + 64], start=False, stop=True)
                        pu = pb[4 + cc]
                        rws = slice(64 * cc, 64 * cc + 64)
                        K.mm(pu[:, 0:256], lhsT=kd[rws, ts_, hp * 128:(hp + 1) * 128], rhs=vtm[rws, ts_, hp * 256:(hp + 1) * 256])
                        K.stt(st2[:, hp, :], st[:, hp, :], Eq[:, hp, cs + 63:cs + 64], pu[:, 0:256], ALU.mult, ALU.add)
                        gsi[hp] = 1 - gsi[hp]
                for j in range(2):
                    h = 2 * hp + j
                    K.act(osb[:, :], po[j][:, :], AF.Copy)
                    K.act(sq[:, :], po[j][:, :], AF.Square)
                    pm = pb[6]
                    K.mm(pm[:, :], lhsT=omean, rhs=sq[:, :])
                    K.act(rs[:, :], pm[:, :], AF.Sqrt, bias=epsb[:, 1:2])
                    K.recip(rs[:, :], rs[:, :])
                    K.tt(osb[:, :], osb[:, :], rs[:, :], ALU.mult)
                    K.stt(oT[1][:, h, :], osb[:, :], par[:, P_GLAN:P_GLAN + 1], sgT[:, h, :], ALU.mult, ALU.mult)
            K.barrier()

    def diff_phase(ti, oT):
        t0 = ti * TT
        nprev = ti * 4
        with ExitStack() as e3:
            QT = loc(e3, "QT", [128, 4, TT], BF16); KTc = loc(e3, "KTc", [128, 4, TT], BF16)
            Vc = loc(e3, "Vc", [128, 4, 512], BF16)
            Kh = [loc(e3, f"Kh{i}", [128, max(1, ti) * TT], BF16) for i in range(2)]
            Vh = [loc(e3, f"Vh{i}", [128, max(1, ti) * 4, 128], BF16) for i in range(2)]
            PT = [loc(e3, f"PT{i}", [128, TT], BF16) for i in range(3)]
            dtmp = loc(e3, "dtmp", [128, 128])
            rr = [loc(e3, f"rr{m}", [128, TT]) for m in range(2)]
            pin = loc(e3, "pin", [128, TT]); den = loc(e3, "den", [128, TT])
            slot = W.next("dQ")
            for h in range(4):
                p_ = pb[h % 2]; proj_fm(p_, slot, h * 128, 128)
                K.act(QT[:, h, :], p_[:, :], AF.Copy, scale=0.125)
            slot = W.next("dK")
            for h in range(4):
                p_ = pb[2 + h % 2]; proj_fm(p_, slot, h * 128, 128)
                ev(h, KTc[:, h, :], p_[:, :])
            slot = W.next("dV")
            for ts_ in range(4):
                p_ = pb[4 + ts_ % 2]; proj_tm(p_, slot, 0, 512, ts_)
                ev(ts_, Vc[:, ts_, :], p_[:, :])
            for h in range(4):
                K.dma("pool", KS[h][:, t0:t0 + TT], KTc[:, h, :], key=f"ksst{h}")
                K.dma("pool", VS[h][:, ti * 4:(ti + 1) * 4, :], Vc[:, :, h * 128:(h + 1) * 128], key=f"vsst{h}")
            cnt = 0
            for h in range(4):
                kh = Kh[h % 2]; vh = Vh[h % 2]
                if ti > 0:
                    K.dma("pool", kh[:, 0:t0], KS[h][:, 0:t0])
                    K.dma("pool", vh[:, 0:nprev, :], VS[h][:, 0:nprev, :])
                for m in range(2):
                    ms = slice(64 * m, 64 * m + 64)
                    po_, pl_ = pb[0], pb[1]
                    for kj in range(nprev):
                        ps_ = pb[2 + kj % 2]
                        K.mm(ps_[:, :], lhsT=kh[ms, kj * 128:(kj + 1) * 128], rhs=QT[ms, h, :])
                        pt_ = PT[cnt % 3]; cnt += 1
                        dd = nprev - kj
                        K.act(pt_[:, :], ps_[:, :], AF.Exp, bias=cst[:, K_BTAB + h * 32 + dd:K_BTAB + h * 32 + dd + 1])
                        K.mm(po_[:, :], lhsT=vh[:, kj, :], rhs=pt_[:, :], start=(kj == 0), stop=(kj == nprev - 1))
                        K.mm(pl_[:, :], lhsT=onesb[:, :], rhs=pt_[:, :], start=(kj == 0), stop=(kj == nprev - 1))
                    pio, pil = pb[4], pb[5]
                    for qi in range(4):
                        qs = slice(qi * 128, (qi + 1) * 128)
                        for kjr in range(qi + 1):
                            ps_ = pb[2 + kjr % 2]
                            K.mm(ps_[:, 0:128], lhsT=KTc[ms, h, kjr * 128:(kjr + 1) * 128], rhs=QT[ms, h, qs])
                            pt_ = PT[cnt % 3]; cnt += 1
                            if kjr < qi:
                                dd = qi - kjr
                                K.act(pt_[:, 0:128], ps_[:, 0:128], AF.Exp, bias=cst[:, K_BTAB + h * 32 + dd:K_BTAB + h * 32 + dd + 1])
                            else:
                                K.tt(dtmp[:, :], ps_[:, 0:128], cst[:, K_BDIAG + h * 128:K_BDIAG + (h + 1) * 128], ALU.add)
                                K.act(pt_[:, 0:128], dtmp[:, :], AF.Exp)
                            K.mm(pio[:, qs], lhsT=Vc[:, kjr, h * 128:(h + 1) * 128], rhs=pt_[:, 0:128], start=(kjr == 0), stop=(kjr == qi))
                            K.mm(pil[:, qs], lhsT=onesb[:, :], rhs=pt_[:, 0:128], start=(kjr == 0), stop=(kjr == qi))
                    K.act(pin[:, :], pio[:, :], AF.Copy)
                    K.act(den[:, :], pil[:, :], AF.Copy)
                    if ti > 0:
                        for qi in range(4):
                            qs = slice(qi * 128, (qi + 1) * 128)
                            f = math.exp(-SLOPES[h] * 128 * qi)
                            K.stt(pin[:, qs], po_[:, qs], f, pin[:, qs], ALU.mult, ALU.add)
                            K.stt(den[:, qs], pl_[:, qs], f, den[:, qs], ALU.mult, ALU.add)
                    K.recip(den[:, :], den[:, :])
                    K.tt(rr[m][:, :], pin[:, :], den[:, :], ALU.mult)
                K.stt(rr[0][:, :], rr[1][:, :], lam[:, 0:1], rr[0][:, :], ALU.mult, ALU.add)
                K.act(pin[:, :], rr[0][:, :], AF.Square)
                pm = pb[6]
                K.mm(pm[:, :], lhsT=omean, rhs=pin[:, :])
                K.act(den[:, :], pm[:, :], AF.Sqrt, bias=epsb[:, 1:2])
                K.recip(den[:, :], den[:, :])
                K.tt(rr[0][:, :], rr[0][:, :], den[:, :], ALU.mult)
                K.ts(oT[2][:, h, :], rr[0][:, :], lam[:, 3:4], ALU.mult)
            K.barrier()

    def rwkv_phase(ti, l, oT):
        t0 = ti * TT
        with ExitStack() as e3:
            vT = loc(e3, "vT", [128, 4, TT]); tanhT = loc(e3, "tanhT", [96, TT]); alrT = loc(e3, "alrT", [96, TT])
            sgl = loc(e3, "sgl", [128, 2, TT])
            tb = [[loc(e3, f"t1{i}", [128, TT]), loc(e3, f"t2{i}", [128, TT])] for i in range(2)]
            zc = [0]

            def zmix(dst, slot, c0, ncols, mucol):
                i = zc[0] % 2; zc[0] += 1
                pa, pq = pb[2 * i], pb[2 * i + 1]
                t1, t2 = tb[i]
                proj_fm(pa, slot, c0, ncols, 0)
                proj_fm(pq, slot, c0, ncols, 1)
                K.act(t1[0:ncols, :], pa[0:ncols, :], AF.Copy)
                K.tt(t2[0:ncols, :], pq[0:ncols, :], t1[0:ncols, :], ALU.subtract)
                K.stt(dst, t2[0:ncols, :], par[0:ncols, mucol:mucol + 1], t1[0:ncols, :], ALU.mult, ALU.add)

            slot = W.next("rV")
            for hp in range(4):
                zmix(vT[:, hp, :], slot, hp * 128, 128, P_MUV + hp)
            if l == 0:
                K.dma("pool", s_vf[:, :, t0:t0 + TT], vT[:, :, :], key="vfst")
            else:
                vf = tb[1][1]; lo1 = tb[1][0]
                p_ = pb[4]
                for kc in range(4):
                    K.mm(p_[0:64, :], lhsT=v1[:, kc, :], rhs=vT[:, kc, :], start=(kc == 0), stop=(kc == 3))
                K.act(lo1[0:64, :], p_[0:64, :], AF.Copy)
                t1, t2 = tb[0]
                for hp in range(4):
                    K.dma("pool", vf[:, :], s_vf[:, hp, t0:t0 + TT])
                    p2 = pb[5 + hp % 2]
                    K.mm(p2[:, :], lhsT=v2[0:64, hp * 128:(hp + 1) * 128], rhs=lo1[0:64, :])
                    K.act(t1[:, :], p2[:, :], AF.Sigmoid, bias=par[:, P_V0 + hp:P_V0 + hp + 1])
                    K.tt(t2[:, :], vf[:, :], vT[:, hp, :], ALU.subtract)
                    K.tt(t2[:, :], t2[:, :], t1[:, :], ALU.mult)
                    K.tt(vT[:, hp, :], vT[:, hp, :], t2[:, :], ALU.add)
            slot = W.next("rL")
            zmix(tanhT[:, :], slot, 0, 96, P_MUW)
            K.act(tanhT[:, :], tanhT[:, :], AF.Tanh)
            zmix(alrT[:, :], slot, 96, 96, P_MUA)
            for c in range(2):
                zmix(sgl[:, c, :], slot, 192 + c * 128, 128, P_MUG + c)
                K.act(sgl[:, c, :], sgl[:, c, :], AF.Sigmoid)

            for hp in range(4):
                with ExitStack() as e4:
                    L2 = lambda nm: loc(e4, nm, [128, TT])
                    rT, kT, aT, kk, bb, bon, Ein, Eip, Eex, Erv = [L2(n) for n in ("rT", "rkT", "aT", "kk", "bb", "bon", "Ein", "Eip", "Eex", "Erv")]
                    ewtm = loc(e4, "ewtm", [128, 4, 128])
                    KKtm, KGtm, BGtm, vtm = [L2(n) for n in ("KKtm", "KGtm", "BGtm", "rvtm")]
                    S2 = lambda nm: loc(e4, nm, [128, 128])
                    bsets = [{n: S2(f"{n}s{si}") for n in ("X", "N", "AkkT", "ArkT", "ArbT", "Y", "nQP", "Reff", "Oloc", "McT", "Gsb", "A0", "A1", "N0", "N1", "X0", "X1")} for si in range(3)]
                    t1, t2 = tb[0]
                    slot = W.next(f"rRK{hp}")
                    zmix(rT[:, :], slot, 0, 128, P_MUR + hp)
                    zmix(kT[:, :], slot, 128, 128, P_MUK + hp)
                    pcol = lambda c: par[:, c + hp:c + hp + 1]
                    p_ = pb[4]
                    K.mm(p_[:, :], lhsT=a2[0:96, hp * 128:(hp + 1) * 128], rhs=alrT[0:96, :])
                    K.act(aT[:, :], p_[:, :], AF.Sigmoid, bias=pcol(P_A0))
                    K.ts(kk[:, :], kT[:, :], pcol(P_KK), ALU.mult)
                    K.act(t1[:, :], kk[:, :], AF.Square)
                    p_ = pb[5]
                    K.mm(p_[:, :], lhsT=bd64, rhs=t1[:, :])
                    K.act(t2[:, :], p_[:, :], AF.Sqrt)
                    K.ts(t2[:, :], t2[:, :], 1e-12, ALU.max)
                    K.recip(t2[:, :], t2[:, :])
                    K.tt(kk[:, :], kk[:, :], t2[:, :], ALU.mult)
                    K.tt(bb[:, :], kk[:, :], aT[:, :], ALU.mult)
                    K.ts(t1[:, :], aT[:, :], -1.0, ALU.add)
                    K.ts(t1[:, :], t1[:, :], pcol(P_KA), ALU.mult)
                    K.ts(t1[:, :], t1[:, :], 1.0, ALU.add)
                    K.tt(kT[:, :], kT[:, :], t1[:, :], ALU.mult)
                    K.tt(bon[:, :], rT[:, :], kT[:, :], ALU.mult)
                    K.ts(bon[:, :], bon[:, :], pcol(P_RK), ALU.mult)
                    for ts_ in range(4):
                        p_ = pb[4 + ts_ % 2]
                        K.mm(p_[:, 0:128], lhsT=tanhT[0:96, ts_ * 128:(ts_ + 1) * 128], rhs=w2[0:96, hp * 128:(hp + 1) * 128], start=True, stop=False)
                        K.mm(p_[:, 0:128], lhsT=ones[0:1, :], rhs=rows[0:1, RW_W0 + hp * 128:RW_W0 + (hp + 1) * 128], start=False, stop=True)
                        K.act(ewtm[:, ts_, :], p_[:, 0:128], AF.Sigmoid)
                    pin_, pex, prv = pb[0], pb[1], pb[2]
                    for ts_ in range(4):
                        cs = slice(ts_ * 128, (ts_ + 1) * 128)
                        K.mm(pin_[:, cs], lhsT=ewtm[:, ts_, :], rhs=mLE)
                        K.mm(pex[:, cs], lhsT=ewtm[:, ts_, :], rhs=mLT)
                        K.mm(prv[:, cs], lhsT=ewtm[:, ts_, :], rhs=mGT)
                    K.act(Ein[:, :], pin_[:, :], AF.Exp, scale=-S0)
                    K.act(Eip[:, :], pin_[:, :], AF.Exp, scale=S0)
                    K.act(Eex[:, :], pex[:, :], AF.Exp, scale=-S0)
                    K.act(Erv[:, :], prv[:, :], AF.Exp, scale=-S0)
                    K.tt(rT[:, :], rT[:, :], Ein[:, :], ALU.mult)
                    K.tt(kk[:, :], kk[:, :], Eex[:, :], ALU.mult)
                    K.tt(aT[:, :], bb[:, :], Erv[:, :], ALU.mult)
                    K.tt(Erv[:, :], kT[:, :], Erv[:, :], ALU.mult)
                    K.tt(bb[:, :], bb[:, :], Eip[:, :], ALU.mult)
                    K.tt(kT[:, :], kT[:, :], Eip[:, :], ALU.mult)
                    Rt, KKt, BG, KG, Bh, Khh = rT, kk, aT, Erv, bb, kT
                    oraw, cen = Eex, Eip
                    for idx, (src_, dst_) in enumerate(((KKt, KKtm), (KG, KGtm), (BG, BGtm), (vT[:, hp, :], vtm))):
                        p_ = pb[3 + idx % 2]
                        for ts_ in range(4):
                            cs = slice(ts_ * 128, (ts_ + 1) * 128)
                            K.tr(p_[:, cs], src_[:, cs], ident)
                        ev(idx, dst_[:, :], p_[:, :])
                    def block(ts_, j, B):
                        cs = slice(ts_ * 128, (ts_ + 1) * 128)
                        hs = slice(64 * j, 64 * j + 64)
                        hc = slice(ts_ * 128 + 64 * j, ts_ * 128 + 64 * j + 64)
                        X, N, AkkT, ArkT, ArbT, Y, nQP, Reff, Oloc, McT, Gsb = [B[n] for n in ("X", "N", "AkkT", "ArkT", "ArbT", "Y", "nQP", "Reff", "Oloc", "McT", "Gsb")]
                        Ab = [B["A0"], B["A1"]]; Nb = [B["N0"], B["N1"]]; Xb = [B["X0"], B["X1"]]

                        def prod(dst, lh, rh, mask):
                            p_ = rps()
                            K.mm(p_[:, 0:128], lhsT=lh[hs, cs], rhs=rh[hs, cs])
                            K.tt(dst[:, :], p_[:, 0:128], mask, ALU.mult)
                        prod(X, Bh, KKt, mLT); prod(N, KKt, Bh, mGT)
                        A = Ab[0]
                        K.tt(A[:, :], ident, X[:, :], ALU.subtract)
                        yield
                        prod(AkkT, Khh, KKt, mLT); prod(ArkT, Khh, Rt, mLE); prod(ArbT, Bh, Rt, mLE)
                        Xc, Nc = X, N
                        for lev in range(5):
                            pN = rps(); K.mm(pN[:, 0:128], lhsT=Xc[:, :], rhs=Nc[:, :])
                            N2 = Nb[lev % 2]; K.act(N2[:, :], pN[:, 0:128], AF.Copy)
                            X2 = Xb[lev % 2]
                            if lev < 4:
                                pX = rps(); K.mm(pX[:, 0:128], lhsT=Nc[:, :], rhs=Xc[:, :])
                                K.copy(X2[:, :], pX[:, 0:128])
                            yield
                            pA = rps(); K.mm(pA[:, 0:128], lhsT=N2[:, :], rhs=A[:, :])
                            A2 = Ab[(lev + 1) % 2]
                            K.tt(A2[:, :], pA[:, 0:128], A[:, :], ALU.add)
                            A = A2; Xc, Nc = X2, N2
                            yield
                        pY = rps(); K.mm(pY[:, 0:64], lhsT=AkkT[:, :], rhs=vtm[:, hc])
                        K.act(Y[:, 0:64], pY[:, 0:64], AF.Copy)
                        yield
                        pQ = rps()
                        K.mm(pQ[:, 0:64], lhsT=A[:, :], rhs=Y[:, 0:64])
                        K.mm(pQ[:, 64:128], lhsT=A[:, :], rhs=KKtm[:, hc])
                        K.act(nQP[:, :], pQ[:, 0:128], AF.Copy, scale=-1.0)
                        yield
                        pR = rps(); K.mm(pR[hs, 0:128], lhsT=nQP[:, 64:128], rhs=ArbT[:, :])
                        K.tt(Reff[hs, :], pR[hs, 0:128], Rt[hs, cs], ALU.add)
                        pO = rps()
                        K.mm(pO[hs, 0:128], lhsT=vtm[:, hc], rhs=ArkT[:, :], start=True, stop=False)
                        K.mm(pO[hs, 0:128], lhsT=nQP[:, 0:64], rhs=ArbT[:, :], start=False, stop=True)
                        K.act(Oloc[hs, :], pO[hs, 0:128], AF.Copy)
                        yield
                        for cc in range(2):
                            cr = slice(64 * cc, 64 * cc + 64); col = ts_ * 128 + 64 * cc
                            pM = rps()
                            K.mm(pM[hs, 0:64], lhsT=nQP[cr, 64:128], rhs=BGtm[cr, hc])
                            K.stt(McT[hs, 64 * cc:64 * cc + 64], ident[hs, 64 * j:64 * j + 64], Ein[hs, col + 63:col + 64], pM[hs, 0:64], ALU.mult, ALU.add)
                            K.mm(pM[hs, 64:128], lhsT=KGtm[cr, hc], rhs=vtm[cr, hc], start=True, stop=False)
                            K.mm(pM[hs, 64:128], lhsT=BGtm[cr, hc], rhs=nQP[cr, 0:64], start=False, stop=True)
                            K.act(Gsb[hs, 64 * cc:64 * cc + 64], pM[hs, 64:128], AF.Copy)
                        yield
                        for cc in range(2):
                            col = ts_ * 128 + 64 * cc
                            key = (hp, j)
                            cur = rsi.get(key, 0)
                            Hc = rst[cur]; Hn = rst[1 - cur]
                            pOo = rps()
                            K.mm(pOo[hs, 0:64], lhsT=Hc[hs, hp, :], rhs=Reff[hs, 64 * cc:64 * cc + 64])
                            K.tt(oraw[hs, col:col + 64], pOo[hs, 0:64], Oloc[hs, 64 * cc:64 * cc + 64], ALU.add)
                            pH = rps()
                            K.mm(pH[hs, 0:64], lhsT=McT[hs, 64 * cc:64 * cc + 64], rhs=Hc[hs, hp, :])
                            K.tt(Hn[hs, hp, :], pH[hs, 0:64], Gsb[hs, 64 * cc:64 * cc + 64], ALU.add)
                            rsi[key] = 1 - cur

                    blocks = [(ts_, j) for ts_ in range(4) for j in range(2)]
                    active = []
                    free_sets = list(range(len(bsets)))
                    nxt = 0
                    while nxt < len(blocks) or active:
                        while free_sets and nxt < len(blocks):
                            si = free_sets.pop(0)
                            active.append((block(blocks[nxt][0], blocks[nxt][1], bsets[si]), si))
                            nxt += 1
                        for item in list(active):
                            try:
                                next(item[0])
                            except StopIteration:
                                active.remove(item)
                                free_sets.append(item[1])
                    p_ = pb[0]
                    K.mm(p_[:, :], lhsT=bd64, rhs=oraw[:, :])
                    K.stt(cen[:, :], p_[:, :], -1.0 / 64, oraw[:, :], ALU.mult, ALU.add)
                    K.act(t1[:, :], cen[:, :], AF.Square)
                    p2 = pb[1]
                    K.mm(p2[:, :], lhsT=bd64, rhs=t1[:, :])
                    K.act(t2[:, :], p2[:, :], AF.Sqrt, bias=epsb[:, 2:3], scale=1.0 / 64)
                    K.recip(t2[:, :], t2[:, :])
                    K.tt(cen[:, :], cen[:, :], t2[:, :], ALU.mult)
                    K.ts(cen[:, :], cen[:, :], pcol(P_LNW), ALU.mult, pcol(P_LNB), ALU.add)
                    p3 = pb[2]
                    K.mm(p3[:, :], lhsT=bd64, rhs=bon[:, :])
                    K.tt(t1[:, :], p3[:, :], vT[:, hp, :], ALU.mult)
                    K.tt(cen[:, :], cen[:, :], t1[:, :], ALU.add)
                    p4 = pb[3]
                    for c in range(2):
                        K.mm(p4[:, :], lhsT=g2[:, c, hp * 128:(hp + 1) * 128], rhs=sgl[:, c, :], start=(c == 0), stop=(c == 1))
                    K.tt(oT[3][:, hp, :], cen[:, :], p4[:, :], ALU.mult)
                    K.barrier()
            K.barrier()

    def merge_phase(oT, mergedT):
        with ExitStack() as e3:
            macc = loc(e3, "macc", [128, 4, TT])
            sg = [loc(e3, f"sg{i}", [128, TT]) for i in range(2)]
            tm = [loc(e3, f"tm{i}", [128, TT]) for i in range(2)]
            for jq in range(4):
                for n in range(4):
                    sg_slot = W.next(f"mg{jq}{n}")
                    sb_slot = W.next(f"mb{jq}{n}")
                    for cq in range(4):
                        pg = pb[cq % 2]
                        proj_fm(pg, sg_slot, cq * 128, 128)
                        s_ = sg[cq % 2]
                        K.act(s_[:, :], pg[:, :], AF.Sigmoid)
                        pbr = pb[2 + cq % 2]
                        for kc in range(4):
                            K.mm(pbr[:, :], lhsT=sb_slot[:, kc, cq * 128:(cq + 1) * 128], rhs=oT[n][:, kc, :], start=(kc == 0), stop=(kc == 3))
                        if n == 0:
                            K.tt(macc[:, cq, :], pbr[:, :], s_[:, :], ALU.mult)
                        else:
                            t_ = tm[cq % 2]
                            K.tt(t_[:, :], pbr[:, :], s_[:, :], ALU.mult)
                            K.tt(macc[:, cq, :], macc[:, cq, :], t_[:, :], ALU.add, en="pool")
                for cq in range(4):
                    K.act(mergedT[:, jq * 4 + cq, :], macc[:, cq, :], AF.Copy)
            K.barrier()

    def wout_phase(l, ti, xsrc, mergedT):
        t0 = ti * TT
        with ExitStack() as e3:
            yall = loc(e3, "yall", [128, 4, D])
            i = 0
            for cg in range(4):
                slot = W.next(f"wo{cg}")
                for ts_ in range(4):
                    p_ = pb[ts_]
                    for kc in range(NKC):
                        K.mm(p_[:, :], lhsT=mergedT[:, kc, ts_ * 128:(ts_ + 1) * 128], rhs=slot[:, kc, 0:512], start=(kc == 0), stop=(kc == NKC - 1))
                    ev(i, yall[:, ts_, cg * 512:(cg + 1) * 512], p_[:, :]); i += 1
            gbc = loc(e3, "gbc", [128, D])
            K.dma("pool", gbc[:, :], gpost_d[l])
            xts = [loc(e3, f"xw{i}", [128, D]) for i in range(2)]
            ssw = loc(e3, "ssw", [128, 4])
            junk = loc(e3, "junkw", [128, D], BF16)

            def src(ts_):
                xt = xts[ts_ % 2]
                K.dma("pool", xt[:, :], xsrc[t0 + ts_ * 128:t0 + (ts_ + 1) * 128, :])
                K.op("act", lambda h: h.activation(out=junk[:, :], in_=yall[:, ts_, :], func=AF.Square, accum_out=ssw[:, ts_:ts_ + 1]),
                     [yall[:, ts_, :]], [junk[:, :], ssw[:, :]])
                rstd_from_ss(ssw[:, ts_:ts_ + 1], ssw[:, ts_:ts_ + 1], D, 0)
                K.stt(yall[:, ts_, :], yall[:, ts_, :], ssw[:, ts_:ts_ + 1], gbc[:, :], ALU.mult, ALU.mult)
                K.tt(xt[:, :], xt[:, :], yall[:, ts_, :], ALU.add, en="pool")
                K.dma("pool", s_xm[ts_ * 128:(ts_ + 1) * 128, :], xt[:, :])
                return xt[:, :]
            norm_transpose(src, P_GFFN, e3, "b")
            K.barrier()

    def ffn_phase(l, ti, xdst):
        t0 = ti * TT
        with ExitStack() as e3:
            yall = loc(e3, "yallf", [128, 4, D])
            with ExitStack() as e4:
                actT = loc(e4, "actT", [128, 44, TT], BF16)
                sgb = [loc(e4, f"sgf{i}", [128, TT], BF16) for i in range(2)]
                for j in range(11):
                    sg_ = W.next(f"fg{j}")
                    su_ = W.next(f"fu{j}")
                    for cq in range(4):
                        pg = pb[cq % 2]; proj_fm(pg, sg_, cq * 128, 128)
                        pu = pb[2 + cq % 2]; proj_fm(pu, su_, cq * 128, 128)
                        s_ = sgb[cq % 2]
                        K.act(s_[:, :], pg[:, :], AF.Silu)
                        K.tt(actT[:, j * 4 + cq, :], pu[:, :], s_[:, :], ALU.mult)
                i = 0
                for cg in range(4):
                    for kg, (k0, nk) in enumerate(((0, 16), (16, 16), (32, 12))):
                        slot = W.next(f"fd{cg}{kg}")
                        for ts_ in range(4):
                            for kq in range(nk):
                                kc = k0 + kq
                                K.mm(pb[ts_][:, :], lhsT=actT[:, kc, ts_ * 128:(ts_ + 1) * 128], rhs=slot[:, kq, 0:512], start=(kc == 0), stop=(kc == 43))
                    for ts_ in range(4):
                        ev(i, yall[:, ts_, cg * 512:(cg + 1) * 512], pb[ts_][:, :]); i += 1
                K.barrier()
            gbc = loc(e3, "gbcf", [128, D])
            K.dma("pool", gbc[:, :], gfpost_d[l])
            xts = [loc(e3, f"xf{i}", [128, D]) for i in range(2)]
            ssf = loc(e3, "ssf", [128, 4])
            junk = loc(e3, "junkf", [128, D], BF16)
            for ts_ in range(4):
                xt = xts[ts_ % 2]
                K.dma("pool", xt[:, :], s_xm[ts_ * 128:(ts_ + 1) * 128, :])
                K.op("act", lambda h: h.activation(out=junk[:, :], in_=yall[:, ts_, :], func=AF.Square, accum_out=ssf[:, ts_:ts_ + 1]),
                     [yall[:, ts_, :]], [junk[:, :], ssf[:, :]])
                rstd_from_ss(ssf[:, ts_:ts_ + 1], ssf[:, ts_:ts_ + 1], D, 0)
                K.stt(yall[:, ts_, :], yall[:, ts_, :], ssf[:, ts_:ts_ + 1], gbc[:, :], ALU.mult, ALU.mult)
                K.tt(xt[:, :], xt[:, :], yall[:, ts_, :], ALU.add, en="pool")
                K.dma("pool", xdst[t0 + ts_ * 128:t0 + (ts_ + 1) * 128, :], xt[:, :])
            K.barrier()

    for li, l in enumerate(layers):
        xsrc = x_in if l == 0 else s_x1
        xdst = s_x1 if (l == 0 and L > 1) else y_out
        lam_init = 0.8 - 0.6 * math.exp(-0.3 * l)
        K.barrier()
        K.dma("pool", par[:, :], par_d[l])
        K.dma("pool", rows[:, :], row_d[l])
        K.dma("pool", wa2[:, :], wa2_d[l])
        K.dma("pool", w2[:, :], w2_d[l])
        K.dma("pool", a2[:, :], a2_d[l])
        K.dma("pool", g2[:, :, :], g2_d[l].rearrange("(k p) n -> p k n", p=128))
        if l > 0:
            K.dma("pool", v1[:, :, :], v1_d[l - 1].rearrange("(k p) n -> p k n", p=128))
            K.dma("pool", v2[:, :], v2_d[l - 1])
        with ExitStack() as e2:
            tmp = loc(e2, f"lamtmp", [128, 128])
            K.tt(tmp[:, 0:64], par[:, P_LQ:P_LQ + 64], par[:, P_LQ + 64:P_LQ + 128], ALU.mult)
            K.tt(tmp[:, 64:128], par[:, P_LQ + 128:P_LQ + 192], par[:, P_LQ + 192:P_LQ + 256], ALU.mult)
            K.op("dve", lambda h: h.reduce_sum(out=lam[:, 1:2], in_=tmp[:, 0:64], axis=mybir.AxisListType.X), [tmp[:, :]], [lam[:, :]])
            K.op("dve", lambda h: h.reduce_sum(out=lam[:, 2:3], in_=tmp[:, 64:128], axis=mybir.AxisListType.X), [tmp[:, :]], [lam[:, :]])
            K.act(lam[:, 1:3], lam[:, 1:3], AF.Exp)
            K.tt(lam[:, 0:1], lam[:, 2:3], lam[:, 1:2], ALU.subtract)
            K.ts(lam[:, 0:1], lam[:, 0:1], -lam_init, ALU.add)
            K.ts(lam[:, 3:4], par[:, P_DIFN:P_DIFN + 1], 1.0 - lam_init, ALU.mult)
            K.barrier()
        for t_ in (gst[0], gst[1], rst[0], rst[1], cu_halo):
            K.memset(t_[:], 0.0)
        K.memset(halo[:, :], 0.0)
        gcur = 0
        rcur = 0

        for ti in range(n_tiles):
            t0 = ti * TT
            with ExitStack() as e2:
                xts = [loc(e2, f"xt{i}", [128, D]) for i in range(2)]

                def src(ts_):
                    xt = xts[ts_ % 2]
                    K.dma("pool", xt[:, :], xsrc[t0 + ts_ * 128:t0 + (ts_ + 1) * 128, :])
                    return xt[:, :]
                for kc in range(NKC):
                    K.copy(hT[kc][:, 0:1], halo[:, kc:kc + 1], en="pool")
                norm_transpose(src, P_GPRE, e2, "a")
                for kc in range(NKC):
                    K.copy(halo[:, kc:kc + 1], hT[kc][:, TT:TT + 1], en="pool")
                K.barrier()
            if ti == 0 and li == 0:
                emit_casts(ncast_layer - 8)
            with ExitStack() as e2:
                mergedT = loc(e2, "mergedT", [128, NKC, TT], BF16)
                oT = [loc(e2, f"oT{n}", [128, 4, TT], BF16) for n in range(4)]
                with ExitStack() as e3:
                    Csb = loc(e3, f"Csb", [128, 4, TT])
                    cu = loc(e3, f"cu", [128, 4, TT + 2])
                    yv = loc(e3, f"yv", [128, 4, TT])
                    slot = W.next("cC")
                    for cc in range(4):
                        p_ = pb[cc % 2]
                        proj_fm(p_, slot, cc * 128, 128)
                        K.act(Csb[:, cc, :], p_[:, :], AF.Copy)
                    K.copy(cu[:, :, 0:2], cu_halo[:, :, :], en="pool")
                    slot = W.next("cU")
                    for cc in range(4):
                        p_ = pb[cc % 2]
                        proj_fm(p_, slot, cc * 128, 128)
                        K.tt(cu[:, cc, 2:TT + 2], p_[:, :], Csb[:, cc, :], ALU.mult)
                        cw = lambda j: par[:, P_CONV + cc * 3 + j:P_CONV + cc * 3 + j + 1]
                        K.ts(yv[:, cc, :], cu[:, cc, 0:TT], cw(0), ALU.mult)
                        K.stt(yv[:, cc, :], cu[:, cc, 1:TT + 1], cw(1), yv[:, cc, :], ALU.mult, ALU.add)
                        K.stt(yv[:, cc, :], cu[:, cc, 2:TT + 2], cw(2), yv[:, cc, :], ALU.mult, ALU.add)
                    K.copy(cu_halo[:, :, :], cu[:, :, TT:TT + 2], en="pool")
                    slot = W.next("cB")
                    for cc in range(4):
                        p_ = pb[cc % 2]
                        proj_fm(p_, slot, cc * 128, 128)
                        K.tt(oT[0][:, cc, :], p_[:, :], yv[:, cc, :], ALU.mult)
                    K.barrier()
                if stage >= 3:
                    gla_phase(ti, oT)
                if stage >= 4:
                    diff_phase(ti, oT)
                if stage >= 5:
                    rwkv_phase(ti, l, oT)
                if ti == n_tiles - 1 and li == L - 1:
                    dump("oconv", oT[0][:, :, :], [128, 4, TT], BF16)
                    dump("ogla", oT[1][:, :, :], [128, 4, TT], BF16)
                    dump("odiff", oT[2][:, :, :], [128, 4, TT], BF16)
                    dump("orwkv", oT[3][:, :, :], [128, 4, TT], BF16)
                if stage >= 6:
                    merge_phase(oT, mergedT)
                    wout_phase(l, ti, xsrc, mergedT)
                K.barrier()
            if stage >= 7:
                ffn_phase(l, ti, xdst)
            if li < L - 1:
                emit_casts(ncast_layer if ti == n_tiles - 1 else -(-ncast_layer // max(1, n_tiles - 1)))
    K.barrier()
    es.close()
    return dumps, K


def host_consts():
    c = np.zeros((128, NCST), np.float32)
    i = np.arange(128)
    r, q = i[:, None], i[None, :]
    same = (r // 64) == (q // 64)
    c[:, K_ID:K_ID + 128] = np.eye(128)
    c[:, K_LE:K_LE + 128] = ((r <= q) & same)
    c[:, K_LT:K_LT + 128] = ((r < q) & same)
    c[:, K_GT:K_GT + 128] = ((r > q) & same)
    c[:, K_MEAN:K_MEAN + 128] = 1.0 / 128
    c[:, K_BD:K_BD + 128] = same
    c[:, K_ONE:K_ONE + 128] = 1.0
    for h in range(4):
        s = SLOPES[h]
        kl, ql = r, q
        vis = (kl // 64) <= (ql // 64)
        b = np.where(kl <= ql, s * kl, s * (2 * ql - kl))
        c[:, K_BDIAG + h * 128:K_BDIAG + (h + 1) * 128] = np.where(vis, b, -30000.0)
        for dd in range(32):
            c[:, K_BTAB + h * 32 + dd] = s * (i - 128.0 * dd)
    return c


def host_params(inp):
    par = np.zeros((2, 128, NPAR), np.float32)
    rows = np.zeros((2, 1, NROW), np.float32)

    def pc(v):
        return np.asarray(v, np.float32).reshape(-1, 128).T
    for l in range(2):
        p = par[l]
        p[:, P_GPRE:P_GPRE + 16] = pc(inp["norm_mix_pre"][l])
        p[:, P_GFFN:P_GFFN + 16] = pc(inp["norm_ffn_pre"][l])
        cw = np.asarray(inp["conv_w"][l], np.float32)
        for cc in range(4):
            for j in range(3):
                p[:, P_CONV + cc * 3 + j] = cw[j, cc * 128:(cc + 1) * 128]
        p[:, P_GLAN] = inp["gla_norm"][l]
        p[:, P_DIFN] = inp["diff_norm"][l]
        mu = np.asarray(inp["rw_mu"][l], np.float32)
        p[:, P_MUR:P_MUR + 4] = pc(mu[0:512])
        p[:, P_MUK:P_MUK + 4] = pc(mu[512:1024])
        p[:, P_MUV:P_MUV + 4] = pc(mu[1024:1536])
        p[0:96, P_MUW] = mu[1536:1632]
        p[0:96, P_MUA] = mu[1632:1728]
        p[:, P_MUG:P_MUG + 2] = pc(mu[1728:1984])
        p[:, P_A0:P_A0 + 4] = pc(inp["rw_a0"][l])
        p[:, P_KK:P_KK + 4] = pc(inp["rw_kk"][l])
        p[:, P_KA:P_KA + 4] = pc(inp["rw_ka"][l])
        p[:, P_RK:P_RK + 4] = pc(inp["rw_rk"][l])
        p[:, P_LNW:P_LNW + 4] = pc(inp["rw_lnw"][l])
        p[:, P_LNB:P_LNB + 4] = pc(inp["rw_lnb"][l])
        if l > 0:
            p[:, P_V0:P_V0 + 4] = pc(inp["rw_v0"][l - 1])
        for j, nm in enumerate(("diff_lq1", "diff_lk1", "diff_lq2", "diff_lk2")):
            p[:, P_LQ + j * 64:P_LQ + (j + 1) * 64] = np.asarray(inp[nm][l], np.float32)[None, :]
        rows[l, 0, RW_BA:RW_BA + 256] = inp["gla_ba"][l]
        rows[l, 0, RW_W0:RW_W0 + 512] = inp["rw_w0"][l]
    return par, rows


def make_inmaps(inp, ncores=4, layers=(0, 1), stage=99):
    f = lambda k: np.asarray(inp[k], np.float32)
    c = np.ascontiguousarray
    par, rows = host_params(inp)
    shared = {"cst": host_consts(), "par": par, "rows": rows,
              "gpost_bc": c(np.broadcast_to(f("norm_mix_post")[:, None, :], (2, 128, D))),
              "gfpost_bc": c(np.broadcast_to(f("norm_ffn_post")[:, None, :], (2, 128, D))),
              "gla_wa2": c(f("gla_wa2")), "rw_w2": c(f("rw_w2")), "rw_a2": c(f("rw_a2")), "rw_g2": c(f("rw_g2")),
              "rw_v1": c(f("rw_v1")), "rw_v2": c(f("rw_v2"))}
    for l in layers:
        wi = f("w_in")[l]
        for j in range(8):
            shared[f"win{l}_{j}"] = c(wi[256 * j:256 * (j + 1)])
        for n in range(4):
            shared[f"wbr{l}_{n}"] = c(f("w_branch")[l, n])
        for j in range(2):
            shared[f"wout{l}_{j}"] = c(f("w_out")[l, 1024 * j:1024 * (j + 1)])
        if stage >= 7:
            for j in range(4):
                shared[f"wg{l}_{j}"] = c(f("w_gate")[l, 512 * j:512 * (j + 1)])
                shared[f"wu{l}_{j}"] = c(f("w_up")[l, 512 * j:512 * (j + 1)])
                shared[f"wd{l}_{j}"] = c(f("w_down")[l, 1408 * j:1408 * (j + 1)])
    x = f("x")
    maps = []
    for cix in range(ncores):
        m = dict(shared)
        m["x"] = c(x[cix % 4])
        maps.append(m)
    return maps


def kernel(**inputs):
    nc = bass.Bass("TRN2", target_bir_lowering=False)
    build(nc)
    maps = make_inmaps(inputs, 4)
    res = run_bass_kernel_spmd(nc, maps, core_ids=list(range(4)))
    return np.stack([res.results[b]["y"] for b in range(4)], axis=0).astype(np.float32)
```
